# Optimizing a Trainium2 kernel written in Bass

```python
import math
import jax, jax.numpy as jnp
from jax import lax
import numpy as np

D_MODEL = 2048
BATCH = 4
SEQ = 2048
DEPTH = 4

N_MIXERS = 3
N_HEADS = 16
HEAD_DIM = D_MODEL // N_HEADS
D_FF = 5632
PLE_DIM = 256
Q_BLOCK = 128
MOBA_BLOCK = 256
MOBA_TOP_K = 3
MOBA_Q_CHUNK = 16
S5_GROUP = 16
S5_GROUPS = D_MODEL // S5_GROUP
S5_STATE = 64
RMS_EPS = 1e-6
N_FOX = len(range(0, DEPTH, N_MIXERS))
N_MOBA = len(range(1, DEPTH, N_MIXERS))
N_S5 = len(range(2, DEPTH, N_MIXERS))

kernel_name = "hybrid_fox_moba_s5_macaron_ple"


def _rmsnorm(x, g):
    xf = x.astype(jnp.float32)
    y = xf * lax.rsqrt(jnp.mean(xf * xf, axis=-1, keepdims=True) + RMS_EPS)
    return (y * g.astype(jnp.float32)).astype(x.dtype)


def _swiglu(x, w_in, w_out):
    gate, up = jnp.split(x @ w_in, 2, axis=-1)
    return (jax.nn.silu(gate) * up) @ w_out


def _fox_attention(xn, w_in, b_f, w_o):
    B, S, D = xn.shape
    H, Dh = N_HEADS, HEAD_DIM
    proj = xn @ w_in
    q, k, v, f_logit = jnp.split(proj, [D, 2 * D, 3 * D], axis=-1)
    q = q.reshape(B, S, H, Dh)
    k = k.reshape(B, S, H, Dh)
    v = v.reshape(B, S, H, Dh)
    log_f = jax.nn.log_sigmoid((f_logit + b_f).astype(jnp.float32))
    c = jnp.cumsum(log_f, axis=1).transpose(0, 2, 1)
    scale = Dh ** -0.5
    kpos = jnp.arange(S)

    def q_block(i):
        t0 = i * Q_BLOCK
        qb = lax.dynamic_slice_in_dim(q, t0, Q_BLOCK, axis=1)
        cb = lax.dynamic_slice_in_dim(c, t0, Q_BLOCK, axis=2)
        logits = jnp.einsum('bqhd,bkhd->bhqk', qb, k).astype(jnp.float32) * scale
        logits = logits + cb[..., None] - c[:, :, None, :]
        qpos = t0 + jnp.arange(Q_BLOCK)
        logits = jnp.where(kpos[None, :] <= qpos[:, None], logits, -jnp.inf)
        probs = jax.nn.softmax(logits, axis=-1).astype(v.dtype)
        return jnp.einsum('bhqk,bkhd->bqhd', probs, v)

    out = lax.map(q_block, jnp.arange(S // Q_BLOCK))
    out = out.transpose(1, 0, 2, 3, 4).reshape(B, S, D)
    return out @ w_o


def _moba_attention(xn, w_qkv, w_o):
    B, S, D = xn.shape
    H, Dh, MB = N_HEADS, HEAD_DIM, MOBA_BLOCK
    n_blk = -(-S // MB)
    s_pad = n_blk * MB
    n_sel = min(MOBA_TOP_K, n_blk)
    q, k, v = jnp.split(xn @ w_qkv, 3, axis=-1)
    q = q.reshape(B, S, H, Dh).transpose(0, 2, 1, 3)
    k = k.reshape(B, S, H, Dh).transpose(0, 2, 1, 3)
    v = v.reshape(B, S, H, Dh).transpose(0, 2, 1, 3)
    pad = ((0, 0), (0, 0), (0, s_pad - S), (0, 0))
    kb = jnp.pad(k, pad).reshape(B, H, n_blk, MB, Dh)
    vb = jnp.pad(v, pad).reshape(B, H, n_blk, MB, Dh)
    k_mean = jnp.mean(kb.astype(jnp.float32), axis=3)
    slopes = 2.0 ** (-8.0 * jnp.arange(1, H + 1, dtype=jnp.float32) / H)
    scale = Dh ** -0.5
    b_idx = jnp.arange(B)[:, None, None, None]
    h_idx = jnp.arange(H)[None, :, None, None]
    blk_ids = jnp.arange(n_blk)
    in_blk = jnp.arange(MB)

    def q_chunk(i):
        t0 = i * MOBA_Q_CHUNK
        qc = lax.dynamic_slice_in_dim(q, t0, MOBA_Q_CHUNK, axis=2)
        qpos = t0 + jnp.arange(MOBA_Q_CHUNK)
        own = t0 // MB
        gate = jnp.einsum('bhqd,bhnd->bhqn', qc.astype(jnp.float32), k_mean)
        gate = jnp.where(blk_ids < own, gate, -jnp.inf)
        _, sel = lax.top_k(gate, n_sel)
        sel_ok = sel < own
        k_sel = kb[b_idx, h_idx, sel]
        v_sel = vb[b_idx, h_idx, sel]
        lg_sel = jnp.einsum('bhqd,bhqnkd->bhqnk', qc, k_sel).astype(jnp.float32) * scale
        pos_sel = sel[..., None] * MB + in_blk
        dist_sel = (qpos[None, None, :, None, None] - pos_sel).astype(jnp.float32)
        lg_sel = lg_sel - slopes[None, :, None, None, None] * dist_sel
        lg_sel = jnp.where(sel_ok[..., None], lg_sel, -jnp.inf)
        lg_sel = lg_sel.reshape(B, H, MOBA_Q_CHUNK, n_sel * MB)
        k_own = lax.dynamic_slice_in_dim(kb, own, 1, axis=2)[:, :, 0]
        v_own = lax.dynamic_slice_in_dim(vb, own, 1, axis=2)[:, :, 0]
        lg_own = jnp.einsum('bhqd,bhkd->bhqk', qc, k_own).astype(jnp.float32) * scale
        dist_own = qpos[:, None] - (own * MB + in_blk)[None, :]
        lg_own = lg_own - slopes[None, :, None, None] * dist_own.astype(jnp.float32)
        lg_own = jnp.where(dist_own >= 0, lg_own, -jnp.inf)
        probs = jax.nn.softmax(jnp.concatenate([lg_sel, lg_own], axis=-1), axis=-1).astype(v.dtype)
        p_sel = probs[..., :n_sel * MB].reshape(B, H, MOBA_Q_CHUNK, n_sel, MB)
        p_own = probs[..., n_sel * MB:]
        return (jnp.einsum('bhqnk,bhqnkd->bhqd', p_sel, v_sel)
                + jnp.einsum('bhqk,bhkd->bhqd', p_own, v_own))

    out = lax.map(q_chunk, jnp.arange(S // MOBA_Q_CHUNK))
    out = out.transpose(1, 0, 3, 2, 4).reshape(B, S, D)
    return out @ w_o


def _s5_layer(xn, w_in, a_re, a_im, log_dt, b_re, b_im, c_re, c_im, d_skip, w_glu):
    B, S, D = xn.shape
    G, P, Cg = S5_GROUPS, S5_STATE, S5_GROUP
    f32 = jnp.float32
    u = (xn @ w_in).astype(f32).reshape(B, S, G, Cg)
    A = lax.complex(a_re.astype(f32), a_im.astype(f32))
    dt = jnp.exp(log_dt.astype(f32))[:, None]
    A_bar = jnp.exp(A * dt)
    B_mat = lax.complex(b_re.astype(f32), b_im.astype(f32))
    B_bar = ((A_bar - 1.0) / A)[..., None] * B_mat
    Bu = jnp.einsum('gpc,bsgc->sbgp', B_bar, u.astype(jnp.complex64))
    a_elems = jnp.broadcast_to(A_bar, (S, 1, G, P))

    def combine(left, right):
        a_l, b_l = left
        a_r, b_r = right
        return a_l * a_r, a_r * b_l + b_r

    _, h = lax.associative_scan(combine, (a_elems, Bu), axis=0)
    C_mat = lax.complex(c_re.astype(f32), c_im.astype(f32))
    y = jnp.einsum('gcp,sbgp->bsgc', C_mat, h).real
    y = y + d_skip.astype(f32).reshape(G, Cg) * u
    y = jax.nn.gelu(y.reshape(B, S, D)).astype(xn.dtype)
    val, gate = jnp.split(y @ w_glu, 2, axis=-1)
    return val * jax.nn.sigmoid(gate)


def setup_inputs(seed: int = 0) -> dict:
    key = jax.random.key(seed)
    ks = jax.random.split(key, 24)
    D, H, F = D_MODEL, N_HEADS, D_FF
    G, P, Cg = S5_GROUPS, S5_STATE, S5_GROUP
    nrm = jax.random.normal
    x = nrm(ks[0], (BATCH, SEQ, D), jnp.float32)
    p = nrm(ks[1], (DEPTH, BATCH, SEQ, PLE_DIM), jnp.float32)
    norm_g = 1.0 + 0.05 * nrm(ks[2], (DEPTH, 4, D), jnp.float32)
    final_g = 1.0 + 0.05 * nrm(ks[3], (D,), jnp.float32)
    w_ffn_in = nrm(ks[4], (DEPTH, 2, D, 2 * F), jnp.float32) * D ** -0.5
    w_ffn_out = nrm(ks[5], (DEPTH, 2, F, D), jnp.float32) * F ** -0.5
    w_ple_gate = nrm(ks[6], (DEPTH, D, D), jnp.float32) * D ** -0.5
    w_ple_proj = nrm(ks[7], (DEPTH, PLE_DIM, D), jnp.float32) * PLE_DIM ** -0.5
    fox_w_in = nrm(ks[8], (N_FOX, D, 3 * D + H), jnp.float32) * D ** -0.5
    fox_b_f = jax.random.uniform(ks[9], (N_FOX, H), jnp.float32, 1.0, 6.0)
    fox_w_o = nrm(ks[10], (N_FOX, D, D), jnp.float32) * D ** -0.5
    moba_w_qkv = nrm(ks[11], (N_MOBA, D, 3 * D), jnp.float32) * D ** -0.5
    moba_w_o = nrm(ks[12], (N_MOBA, D, D), jnp.float32) * D ** -0.5
    s5_w_in = nrm(ks[13], (N_S5, D, D), jnp.float32) * D ** -0.5
    s5_a_re = -0.5 + 0.01 * nrm(ks[14], (N_S5, G, P), jnp.float32)
    s5_a_im = jnp.broadcast_to(math.pi * jnp.arange(P, dtype=jnp.float32), (N_S5, G, P))
    s5_log_dt = jax.random.uniform(ks[15], (N_S5, G), jnp.float32, math.log(1e-3), math.log(1e-1))
    s5_b_re = nrm(ks[16], (N_S5, G, P, Cg), jnp.float32) * (2.0 * Cg) ** -0.5
    s5_b_im = nrm(ks[17], (N_S5, G, P, Cg), jnp.float32) * (2.0 * Cg) ** -0.5
    s5_c_re = nrm(ks[18], (N_S5, G, Cg, P), jnp.float32) * (2.0 * P) ** -0.5 * 4.0
    s5_c_im = nrm(ks[19], (N_S5, G, Cg, P), jnp.float32) * (2.0 * P) ** -0.5 * 4.0
    s5_d = nrm(ks[20], (N_S5, D), jnp.float32)
    s5_w_glu = nrm(ks[21], (N_S5, D, 2 * D), jnp.float32) * D ** -0.5
    return {"x": x, "p": p, "norm_g": norm_g, "final_g": final_g,
            "w_ffn_in": w_ffn_in, "w_ffn_out": w_ffn_out,
            "w_ple_gate": w_ple_gate, "w_ple_proj": w_ple_proj,
            "fox_w_in": fox_w_in, "fox_b_f": fox_b_f, "fox_w_o": fox_w_o,
            "moba_w_qkv": moba_w_qkv, "moba_w_o": moba_w_o,
            "s5_w_in": s5_w_in, "s5_a_re": s5_a_re, "s5_a_im": s5_a_im,
            "s5_log_dt": s5_log_dt, "s5_b_re": s5_b_re, "s5_b_im": s5_b_im,
            "s5_c_re": s5_c_re, "s5_c_im": s5_c_im, "s5_d": s5_d, "s5_w_glu": s5_w_glu}


def reference(x, p, norm_g, final_g, w_ffn_in, w_ffn_out, w_ple_gate, w_ple_proj,
              fox_w_in, fox_b_f, fox_w_o, moba_w_qkv, moba_w_o,
              s5_w_in, s5_a_re, s5_a_im, s5_log_dt, s5_b_re, s5_b_im,
              s5_c_re, s5_c_im, s5_d, s5_w_glu):
    h = x
    for i in range(DEPTH):
        g = norm_g[i]
        h = h + 0.5 * _swiglu(_rmsnorm(h, g[0]), w_ffn_in[i, 0], w_ffn_out[i, 0])
        xn = _rmsnorm(h, g[1])
        kind, j = i % N_MIXERS, i // N_MIXERS
        if kind == 0:
            mix = _fox_attention(xn, fox_w_in[j], fox_b_f[j], fox_w_o[j])
        elif kind == 1:
            mix = _moba_attention(xn, moba_w_qkv[j], moba_w_o[j])
        else:
            mix = _s5_layer(xn, s5_w_in[j], s5_a_re[j], s5_a_im[j], s5_log_dt[j],
                            s5_b_re[j], s5_b_im[j], s5_c_re[j], s5_c_im[j], s5_d[j], s5_w_glu[j])
        h = h + mix
        h = h + 0.5 * _swiglu(_rmsnorm(h, g[2]), w_ffn_in[i, 1], w_ffn_out[i, 1])
        ple_gate = jax.nn.sigmoid(_rmsnorm(h, g[3]) @ w_ple_gate[i])
        h = h + ple_gate * (p[i] @ w_ple_proj[i])
    return _rmsnorm(h, final_g)
```

```python
import contextlib
import numpy as np
import concourse.bass as bass
import concourse.mybir as mybir
from concourse.bass_utils import run_bass_kernel_spmd

F32 = mybir.dt.float32
BF16 = mybir.dt.bfloat16
AF = mybir.ActivationFunctionType
ALU = mybir.AluOpType
AX = mybir.AxisListType

ENGS = ("pe", "act", "dve", "pool", "sp")


class Prog:
    def __init__(self, nc):
        self.nc = nc
        self.streams = {e: [] for e in ENGS}
        self.seq = {e: 0 for e in ENGS}
        self.seen = {e: {} for e in ENGS}
        self.clock = {}
        self.res_w = {}
        self.res_r = {}
        self.dma_cnt = {}
        self.stack = contextlib.ExitStack()
        self.n_t = 0

    def sbuf(self, name, shape, dtype):
        self.n_t += 1
        return self.stack.enter_context(self.nc.sbuf_tensor(f"{name}_{self.n_t}", list(shape), dtype))

    def psum(self, name, shape, dtype):
        self.n_t += 1
        return self.stack.enter_context(self.nc.psum_tensor(f"{name}_{self.n_t}", list(shape), dtype))

    def op(self, eng, fn, reads=(), writes=(), dma=None):
        deps = {}

        def add(ev):
            s, v = ev
            if deps.get(s, 0) < v:
                deps[s] = v

        for r in reads:
            ev = self.res_w.get(r)
            if ev is not None:
                add(ev)
        for w in writes:
            ev = self.res_w.get(w)
            if ev is not None:
                add(ev)
            rr = self.res_r.get(w)
            if rr:
                for s, v in rr.items():
                    add((s, v))
        seen = self.seen[eng]
        waits = []
        for s, v in deps.items():
            if eng == "pe" and s == "pe":
                continue
            if seen.get(s, 0) >= v:
                continue
            waits.append((s, v))
        for s, v in waits:
            if seen.get(s, 0) < v:
                seen[s] = v
            snap = self.clock.get((s, v))
            if snap:
                for k, x in snap.items():
                    if seen.get(k, 0) < x:
                        seen[k] = x
        if dma is None:
            self.seq[eng] += 1
            ev = (eng, self.seq[eng])
        else:
            self.dma_cnt[dma] = self.dma_cnt.get(dma, 0) + 16
            ev = (dma, self.dma_cnt[dma])
        self.clock[ev] = dict(seen)
        for r in reads:
            d = self.res_r.setdefault(r, {})
            if d.get(ev[0], 0) < ev[1]:
                d[ev[0]] = ev[1]
        for w in writes:
            self.res_w[w] = ev
            self.res_r[w] = {}
        self.streams[eng].append((waits, fn, ev))
        return ev

    def seal(self, name):
        tot = self.dma_cnt.get(name)
        if tot is None:
            return
        for k, ev in self.res_w.items():
            if ev[0] == name and ev[1] < tot:
                self.res_w[k] = (name, tot)
        for k, dd in self.res_r.items():
            if dd.get(name, tot) < tot:
                dd[name] = tot

    def barrier(self):
        evs = [(e, self.seq[e]) for e in ENGS if self.seq[e] > 0]
        evs += [(s, v) for s, v in self.dma_cnt.items()]
        for e in ENGS:
            seen = self.seen[e]
            waits = []
            for s, v in evs:
                if s == e and e == "pe":
                    continue
                if seen.get(s, 0) >= v:
                    continue
                waits.append((s, v))
                seen[s] = v
            if waits:
                self.streams[e].append((waits, None, None))

    def emit(self):
        nc = self.nc
        needed = {e: set() for e in ENGS}
        for e in ENGS:
            for waits, fn, ev in self.streams[e]:
                for s, v in waits:
                    if s in needed:
                        needed[s].add(v)
        rank = {}
        for e in ENGS:
            for i, v in enumerate(sorted(needed[e])):
                rank[(e, v)] = i + 1
        sem_names = list(ENGS) + sorted(self.dma_cnt.keys())
        sems = {}
        for s in sem_names:
            sems[s] = self.stack.enter_context(nc.semaphore(f"s_{s}"))
        handles = {"pe": "tensor", "act": "scalar", "dve": "vector", "pool": "gpsimd", "sp": "sync"}
        streams = self.streams
        with nc.Block() as block:
            def make(e):
                def body(eng):
                    for waits, fn, ev in streams[e]:
                        for s, v in waits:
                            val = rank[(s, v)] if s in needed else v
                            eng.wait_ge(sems[s], val)
                        if fn is None:
                            continue
                        ins = fn(eng)
                        if ev[0] in needed:
                            if ev[1] in needed[ev[0]]:
                                ins.then_inc(sems[ev[0]], 1)
                        else:
                            ins.then_inc(sems[ev[0]], 16)
                return body
            for e in ENGS:
                if streams[e]:
                    getattr(block, handles[e])(make(e))


D = 2048
KC = D // 128
FF = 5632
FC = FF // 128
NQ = 4
FQ = FC // NQ
H = 16
DH = 128
SEQ = 2048
BATCH = 4
DEPTH = 4
PLE = 256
EPS = 1e-6
T = 1024
TH = 512
WSLOT = 4096
NWS = 4


class Ctx:
    def __init__(self, P, wring=True):
        self.P = P
        nc = P.nc
        self.ps = P.psum("ps", [128, 8, TH], F32)
        self.bank_i = 0
        self.wring = [P.sbuf(f"wr{i}", [128, WSLOT], BF16) for i in range(NWS)] if wring else []
        self.w_i = 0
        self.ones = P.sbuf("ones", [128, 128], BF16)
        P.op("dve", lambda e: e.memset(self.ones[:], 1.0), writes=["ones"])
        self.eps = P.sbuf("eps", [128, 1], F32)
        P.op("dve", lambda e: e.memset(self.eps[:], EPS), writes=["eps"])

    def bank(self):
        b = self.bank_i
        self.bank_i = (b + 1) % 8
        return b

    def load_w(self, src, kc, gc):
        P = self.P
        s = self.w_i
        self.w_i = (s + 1) % NWS
        n = kc * gc
        assert n <= WSLOT
        slot = self.wring[s]
        piece = 2048 if n % 2048 == 0 else (n if n <= 2048 else None)
        if piece is None:
            for c in range(2048, 0, -1):
                if n % c == 0:
                    piece = c
                    break
        npieces = n // piece
        dst = slot[:, 0:n].rearrange("p (a b) -> p a b", b=piece)
        srcv = src.rearrange("p (a b) -> p a b", b=piece)
        P.op("pool", lambda e: e.dma_start(out=dst, in_=srcv), writes=[("w", s)], dma=f"w{s}")
        return slot[:, 0:n].rearrange("p (k c) -> p k c", c=gc), ("w", s)


def rmsnorm(C, hT, gcol, xn, rstd, sq, hkey, xkey):
    P = C.P
    banks = [C.bank(), C.bank()]
    for kc in range(KC):
        s = sq[kc % 2]
        P.op("act", lambda e, kc=kc, s=s: e.activation(out=s[:], in_=hT[:, kc, :], func=AF.Square),
             reads=[(hkey, kc)], writes=[("sq", kc % 2)])
        for th in range(2):
            P.op("pe", lambda e, kc=kc, s=s, th=th: e.matmul(
                C.ps[:, banks[th], :], lhsT=C.ones[:], rhs=s[:, th * TH:(th + 1) * TH],
                start=(kc == 0), stop=(kc == KC - 1)),
                reads=[("sq", kc % 2), "ones"], writes=[("ps", banks[th])])
    for th in range(2):
        P.op("act", lambda e, th=th: e.activation(
            out=rstd[:, th * TH:(th + 1) * TH], in_=C.ps[:, banks[th], :], func=AF.Sqrt,
            scale=1.0 / D, bias=C.eps[:]),
            reads=[("ps", banks[th]), "eps"], writes=[("rstd", th)])
        P.op("dve", lambda e, th=th: e.reciprocal(
            out=rstd[:, th * TH:(th + 1) * TH], in_=rstd[:, th * TH:(th + 1) * TH]),
            reads=[("rstd", th)], writes=[("rstd", th)])
    for kc in range(KC):
        P.op("dve", lambda e, kc=kc: e.scalar_tensor_tensor(
            out=xn[:, kc, :], in0=hT[:, kc, :], scalar=gcol[:, kc:kc + 1], in1=rstd[:],
            op0=ALU.mult, op1=ALU.mult),
            reads=[(hkey, kc), ("rstd", 0), ("rstd", 1), "gains"], writes=[(xkey, kc)])


def ffn(C, hT, xn, hid, sg, w_in, w_out, hkey, xkey):
    P = C.P
    for q in range(NQ):
        for jj in range(FQ):
            j = q * FQ + jj
            w, wk = C.load_w(w_in[j], KC, 256)
            banks = [[C.bank(), C.bank()], [C.bank(), C.bank()]]
            for kc in range(KC):
                for gu in range(2):
                    for th in range(2):
                        P.op("pe", lambda e, kc=kc, gu=gu, th=th, w=w, banks=banks: e.matmul(
                            C.ps[:, banks[gu][th], :], lhsT=w[:, kc, gu * 128:(gu + 1) * 128],
                            rhs=xn[:, kc, th * TH:(th + 1) * TH], start=(kc == 0), stop=(kc == KC - 1)),
                            reads=[wk, (xkey, kc)], writes=[("ps", banks[gu][th])])
            for th in range(2):
                s = sg[th]
                P.op("act", lambda e, th=th, s=s, banks=banks: e.activation(
                    out=s[:], in_=C.ps[:, banks[0][th], :], func=AF.Silu),
                    reads=[("ps", banks[0][th])], writes=[("sg", th)])
                P.op("dve", lambda e, th=th, s=s, jj=jj, banks=banks: e.tensor_tensor(
                    out=hid[:, jj, th * TH:(th + 1) * TH], in0=C.ps[:, banks[1][th], :], in1=s[:],
                    op=ALU.mult),
                    reads=[("ps", banks[1][th]), ("sg", th)], writes=[("hid", jj, th)])
        for og in range(8):
            w, wk = C.load_w(w_out[q, og], FQ, 256)
            for o2 in range(2):
                dc = og * 2 + o2
                banks = [C.bank(), C.bank()]
                for jj in range(FQ):
                    for th in range(2):
                        P.op("pe", lambda e, jj=jj, th=th, o2=o2, w=w, banks=banks: e.matmul(
                            C.ps[:, banks[th], :], lhsT=w[:, jj, o2 * 128:(o2 + 1) * 128],
                            rhs=hid[:, jj, th * TH:(th + 1) * TH], start=(jj == 0), stop=(jj == FQ - 1)),
                            reads=[wk, ("hid", jj, th)], writes=[("ps", banks[th])])
                for th in range(2):
                    P.op("dve", lambda e, th=th, dc=dc, banks=banks: e.scalar_tensor_tensor(
                        out=hT[:, dc, th * TH:(th + 1) * TH], in0=C.ps[:, banks[th], :], scalar=0.5,
                        in1=hT[:, dc, th * TH:(th + 1) * TH], op0=ALU.mult, op1=ALU.add),
                        reads=[("ps", banks[th]), (hkey, dc)], writes=[(hkey, dc)])


def linear(C, x, xkey, nk, w_dram, n_groups, evac, gc=256):
    P = C.P
    for og in range(n_groups):
        w, wk = C.load_w(w_dram[og], nk, gc)
        banks = [[C.bank(), C.bank()] for _ in range(gc // 128)]
        for kc in range(nk):
            for o2 in range(gc // 128):
                for th in range(2):
                    P.op("pe", lambda e, kc=kc, o2=o2, th=th, w=w, banks=banks: e.matmul(
                        C.ps[:, banks[o2][th], :], lhsT=w[:, kc, o2 * 128:(o2 + 1) * 128],
                        rhs=x[:, kc, th * TH:(th + 1) * TH], start=(kc == 0), stop=(kc == nk - 1)),
                        reads=[wk, (xkey, kc)], writes=[("ps", banks[o2][th])])
        evac(og, banks)


def add_into_h(C, hT, hkey, tmp_fn):
    pass


def build_R(kind_in, kind_out, n_norm):
    nc = bass.Bass("TRN2", target_bir_lowering=False)

    def din(name, shape, dt=F32):
        return nc.dram_tensor(name, list(shape), dt, kind="ExternalInput").ap()

    def dout(name, shape, dt=F32):
        return nc.dram_tensor(name, list(shape), dt, kind="ExternalOutput").ap()

    h_in = din("h_in", [128, KC, T])
    gains_d = din("gains", [128, n_norm * KC])
    if kind_in is not None:
        mix_in = din("mix_in", [128, KC, T], BF16)
        if kind_in == "attn":
            wo_d = din("wo", [8, 128, KC * 256])
        else:
            wglu_d = din("wglu", [16, 128, KC * 256])
        f2in_d = din("f2_in", [FC, 128, KC * 256])
        f2out_d = din("f2_out", [NQ, 8, 128, FQ * 256])
        pleg_d = din("ple_g", [8, 128, KC * 256])
        plep_d = din("ple_p", [1, 128, 2 * D])
        pT_d = din("pT", [128, 2, T])
    if kind_out != "final":
        f1in_d = din("f1_in", [FC, 128, KC * 256])
        f1out_d = din("f1_out", [NQ, 8, 128, FQ * 256])
        h_out = dout("h_out", [128, KC, T])
    if kind_out in ("fox", "moba"):
        wq_d = din("wq", [8, 128, KC * 256])
        wk_d = din("wk", [8, 128, KC * 256])
        wv_d = din("wv", [8, 128, KC * 256])
        qT_o = dout("qT", [128, H, T], BF16)
        kT_o = dout("kT", [128, H, T], BF16)
        v_o = dout("v", [128, T // 128, D], BF16)
    if kind_out == "fox":
        wf_d = din("wf", [1, 128, KC * 16])
        bf_d = din("bf_bc", [128, H])
        logf_o = dout("logf", [128, T // 128, H])
    if kind_out == "s5":
        wu_d = din("wu", [8, 128, KC * 256])
        uT_o = dout("uT", [128, KC, T], BF16)
    if kind_out == "final":
        out_o = dout("out", [128, KC, T])

    P = Prog(nc)
    with P.stack:
        C = Ctx(P)
        hT = P.sbuf("hT", [128, KC, T], F32)
        xn = P.sbuf("xn", [128, KC, T], BF16)
        hid = P.sbuf("hid", [128, KC, T], BF16)
        sg = [P.sbuf("sg", [128, TH], F32) for _ in range(2)]
        sq = [P.sbuf("sq", [128, T], BF16) for _ in range(2)]
        rstd = P.sbuf("rstd", [128, T], F32)
        gains = P.sbuf("gains", [128, n_norm * KC], F32)
        st = [P.sbuf("st", [128, 2 * T], BF16) for _ in range(2)]
        wp_buf = P.sbuf("wp_buf", [128, 2, D], BF16)
        st_i = [0]
        one_c = P.sbuf("one_c", [128, 1], F32)
        P.op("dve", lambda e: e.memset(one_c[:], 1.0), writes=["one_c"])

        for kc in range(KC):
            P.op("sp", lambda e, kc=kc: e.dma_start(out=hT[:, kc, :], in_=h_in[:, kc, :]),
                 writes=[("h", kc)], dma="ldh")
        P.seal("ldh")
        P.op("sp", lambda e: e.dma_start(out=gains[:], in_=gains_d), writes=["gains"], dma="ldg")
        norm_i = [0]

        def next_norm():
            i = norm_i[0]
            norm_i[0] += 1
            rmsnorm(C, hT, gains[:, i * KC:(i + 1) * KC], xn, rstd, sq, "h", "xn")

        def resid_evac(scale):
            def ev(og, banks):
                for o2 in range(2):
                    dc = og * 2 + o2
                    for th in range(2):
                        P.op("dve", lambda e, th=th, dc=dc, b=banks[o2][th]: e.scalar_tensor_tensor(
                            out=hT[:, dc, th * TH:(th + 1) * TH], in0=C.ps[:, b, :], scalar=scale,
                            in1=hT[:, dc, th * TH:(th + 1) * TH], op0=ALU.mult, op1=ALU.add),
                            reads=[("ps", banks[o2][th]), ("h", dc)], writes=[("h", dc)])
            return ev

        def gated_evac(dc_of):
            def ev(og, banks):
                dc = dc_of(og)
                for th in range(2):
                    s = sg[th]
                    P.op("act", lambda e, th=th, s=s, b=banks[1][th]: e.activation(
                        out=s[:], in_=C.ps[:, b, :], func=AF.Sigmoid),
                        reads=[("ps", banks[1][th])], writes=[("sg", th)])
                    P.op("dve", lambda e, th=th, s=s, b=banks[0][th]: e.tensor_tensor(
                        out=s[:], in0=C.ps[:, b, :], in1=s[:], op=ALU.mult),
                        reads=[("ps", banks[0][th]), ("sg", th)], writes=[("sg", th)])
                    P.op("dve", lambda e, th=th, s=s, dc=dc: e.tensor_tensor(
                        out=hT[:, dc, th * TH:(th + 1) * TH], in0=hT[:, dc, th * TH:(th + 1) * TH],
                        in1=s[:], op=ALU.add),
                        reads=[("sg", th), ("h", dc)], writes=[("h", dc)])
            return ev

        if kind_in is not None:
            for c in range(KC):
                P.op("sp", lambda e, c=c: e.dma_start(out=hid[:, c, :], in_=mix_in[:, c, :]),
                     writes=[("hid", c, 0), ("hid", c, 1)], dma="ldm")
            P.seal("ldm")
            if kind_in == "attn":
                linear_h(C, hid, KC, wo_d, 8, resid_evac(1.0))
            else:
                linear_h(C, hid, KC, wglu_d, 16, gated_evac(lambda og: og))
            next_norm()
            ffn(C, hT, xn, hid, sg, f2in_d, f2out_d, "h", "xn")
            next_norm()
            pT = st
            for k2 in range(2):
                P.op("pool", lambda e, k2=k2: e.dma_start(out=pT[k2][:, 0:T], in_=pT_d[:, k2, :]),
                     writes=[("st", k2)], dma=f"ldp{k2}")
            wp, wpk = wp_buf, "wp"
            P.op("pool", lambda e: e.dma_start(
                out=wp_buf[:],
                in_=plep_d[0].rearrange("p (a b) -> p a b", b=2048)), writes=["wp"], dma="ldwp")

            def ple_evac(og, banks):
                for o2 in range(2):
                    dc = og * 2 + o2
                    pb = [C.bank(), C.bank()]
                    for k2 in range(2):
                        for th in range(2):
                            P.op("pe", lambda e, k2=k2, th=th, dc=dc, pb=pb: e.matmul(
                                C.ps[:, pb[th], :], lhsT=wp[:, k2, dc * 128:(dc + 1) * 128],
                                rhs=pT[k2][:, th * TH:(th + 1) * TH], start=(k2 == 0), stop=(k2 == 1)),
                                reads=[wpk, ("st", k2)], writes=[("ps", pb[th])])
                    gated_evac(lambda og_, dc=dc: dc)(og, [pb, banks[o2]])
            linear(C, xn, "xn", KC, pleg_d, 8, ple_evac)

        if kind_out == "final":
            i = norm_i[0]
            gcol = gains[:, i * KC:(i + 1) * KC]
            rmsnorm(C, hT, gcol, xn, rstd, sq, "h", "xn")
            for kc in range(KC):
                P.op("dve", lambda e, kc=kc: e.scalar_tensor_tensor(
                    out=hT[:, kc, :], in0=hT[:, kc, :], scalar=gcol[:, kc:kc + 1], in1=rstd[:],
                    op0=ALU.mult, op1=ALU.mult),
                    reads=[("h", kc), ("rstd", 0), ("rstd", 1), "gains"], writes=[("h", kc)])
                P.op("sp", lambda e, kc=kc: e.dma_start(out=out_o[:, kc, :], in_=hT[:, kc, :]),
                     reads=[("h", kc)], dma="sto")
        else:
            next_norm()
            ffn(C, hT, xn, hid, sg, f1in_d, f1out_d, "h", "xn")
            for kc in range(KC):
                P.op("sp", lambda e, kc=kc: e.dma_start(out=h_out[:, kc, :], in_=hT[:, kc, :]),
                     reads=[("h", kc)], dma="sth")
            next_norm()

            def store_evac(dst, scale):
                def ev(og, banks):
                    for o2 in range(2):
                        oc = og * 2 + o2
                        s = st_i[0]
                        st_i[0] = 1 - s
                        for th in range(2):
                            P.op("act", lambda e, th=th, s=s, b=banks[o2][th]: e.activation(
                                out=st[s][:, th * TH:(th + 1) * TH], in_=C.ps[:, b, :], func=AF.Copy,
                                scale=scale),
                                reads=[("ps", banks[o2][th])], writes=[("st", s)])
                        P.op("sp", lambda e, s=s, oc=oc: e.dma_start(out=dst[:, oc, :], in_=st[s][:, 0:T]),
                             reads=[("st", s)], dma=f"stq{s}")
                return ev

            if kind_out in ("fox", "moba"):
                linear(C, xn, "xn", KC, wq_d, 8, store_evac(qT_o, DH ** -0.5))
                linear(C, xn, "xn", KC, wk_d, 8, store_evac(kT_o, 1.0))
                for cg in range(8):
                    w, wk = C.load_w(wv_d[cg], KC, 256)
                    s = st_i[0]
                    st_i[0] = 1 - s
                    for tc in range(T // 128):
                        b = C.bank()
                        for kc in range(KC):
                            P.op("pe", lambda e, kc=kc, tc=tc, w=w, b=b: e.matmul(
                                C.ps[:, b, 0:256], lhsT=xn[:, kc, tc * 128:(tc + 1) * 128], rhs=w[:, kc, :],
                                start=(kc == 0), stop=(kc == KC - 1)),
                                reads=[wk, ("xn", kc)], writes=[("ps", b)])
                        P.op("act", lambda e, tc=tc, s=s, b=b: e.activation(
                            out=st[s][:, tc * 256:(tc + 1) * 256], in_=C.ps[:, b, 0:256], func=AF.Copy),
                            reads=[("ps", b)], writes=[("st", s)])
                    P.op("sp", lambda e, s=s, cg=cg: e.dma_start(
                        out=v_o[:, :, cg * 256:(cg + 1) * 256],
                        in_=st[s][:, 0:(T // 128) * 256].rearrange("p (a b) -> p a b", b=256)),
                        reads=[("st", s)], dma=f"stq{s}")
            if kind_out == "fox":
                w, wk = C.load_w(wf_d[0], KC, 16)
                bfb = P.sbuf("bfb", [128, H], F32)
                lf = P.sbuf("lf", [128, T // 128, H], F32)
                P.op("sp", lambda e: e.dma_start(out=bfb[:], in_=bf_d), writes=["bfb"], dma="ldb")
                for tc in range(T // 128):
                    b = C.bank()
                    for kc in range(KC):
                        P.op("pe", lambda e, kc=kc, tc=tc, b=b: e.matmul(
                            C.ps[:, b, 0:H], lhsT=xn[:, kc, tc * 128:(tc + 1) * 128], rhs=w[:, kc, :],
                            start=(kc == 0), stop=(kc == KC - 1)),
                            reads=[wk, ("xn", kc)], writes=[("ps", b)])
                    P.op("dve", lambda e, tc=tc, b=b: e.tensor_tensor(
                        out=lf[:, tc, :], in0=C.ps[:, b, 0:H], in1=bfb[:], op=ALU.add),
                        reads=[("ps", b), "bfb"], writes=[("lf", tc)])
                    P.op("act", lambda e, tc=tc: e.activation(
                        out=lf[:, tc, :], in_=lf[:, tc, :], func=AF.Exp, scale=-1.0),
                        reads=[("lf", tc)], writes=[("lf", tc)])
                    P.op("act", lambda e, tc=tc: e.activation(
                        out=lf[:, tc, :], in_=lf[:, tc, :], func=AF.Ln, bias=one_c[:]),
                        reads=[("lf", tc), "one_c"], writes=[("lf", tc)])
                    P.op("dve", lambda e, tc=tc: e.tensor_scalar(
                        out=lf[:, tc, :], in0=lf[:, tc, :], scalar1=-1.0, scalar2=None, op0=ALU.mult),
                        reads=[("lf", tc)], writes=[("lf", tc)])
                P.op("sp", lambda e: e.dma_start(out=logf_o, in_=lf[:]),
                     reads=[("lf", tc) for tc in range(T // 128)], dma="stl")
            if kind_out == "s5":
                linear(C, xn, "xn", KC, wu_d, 8, store_evac(uT_o, 1.0))
        P.barrier()
        P.emit()
    return nc


def linear_h(C, hid, nk, w_dram, n_groups, evac):
    P = C.P
    for og in range(n_groups):
        w, wk = C.load_w(w_dram[og], nk, 256)
        banks = [[C.bank(), C.bank()] for _ in range(2)]
        for kc in range(nk):
            for o2 in range(2):
                for th in range(2):
                    P.op("pe", lambda e, kc=kc, o2=o2, th=th, w=w, banks=banks: e.matmul(
                        C.ps[:, banks[o2][th], :], lhsT=w[:, kc, o2 * 128:(o2 + 1) * 128],
                        rhs=hid[:, kc, th * TH:(th + 1) * TH], start=(kc == 0), stop=(kc == nk - 1)),
                        reads=[wk, ("hid", kc, th)], writes=[("ps", banks[o2][th])])
        evac(og, banks)


HL = 8
NCH = SEQ // 128
NQT = SEQ // TH
NEG = -30000.0


def build_attn(kind):
    nc = bass.Bass("TRN2", target_bir_lowering=False)

    def din(name, shape, dt=F32):
        return nc.dram_tensor(name, list(shape), dt, kind="ExternalInput").ap()

    qT_d = din("qT", [128, HL, SEQ], BF16)
    kT_d = din("kT", [128, HL, SEQ], BF16)
    v_d = din("v", [128, NCH, HL * DH], BF16)
    tri_d = din("trineg", [128, 128], BF16)
    idb_d = din("ident_bf", [128, 128], BF16)
    sel_d = din("sel", [128, 8 * 128], BF16)
    if kind == "fox":
        logf_d = din("logf", [128, NCH, HL])
        tri32_d = din("tri32", [128, 128])
        ones32_d = din("ones32", [128, 128])
    else:
        ident_d = din("ident32", [128, 128])
        gmask_d = din("gmask", [128, NCH, 8])
        keep_d = din("keep", [128, NCH, 8])
        kq_d = din("kq", [128, NCH, NQT])
        shq_d = din("shq", [8, SEQ])
        slope_d = din("slopes", [128, HL])
    out_d = nc.dram_tensor("attnT", [128, HL, SEQ], BF16, kind="ExternalOutput").ap()

    P = Prog(nc)
    with P.stack:
        C = Ctx(P, wring=False)
        qT = [P.sbuf("qT", [128, SEQ], BF16) for _ in range(2)]
        kT = [P.sbuf("kT", [128, SEQ], BF16) for _ in range(2)]
        vv = [P.sbuf("v", [128, NCH, DH], BF16) for _ in range(2)]
        ao = [P.sbuf("ao", [128, SEQ], BF16) for _ in range(2)]
        pT = [P.sbuf("pT", [128, TH], BF16) for _ in range(3)]
        rl = P.sbuf("rl", [128, TH], F32)
        tri = P.sbuf("tri", [128, 128], BF16)
        sel = P.sbuf("sel", [128, 8 * 128], BF16)
        R = P.sbuf("R", [128, SEQ], BF16)
        P.op("sp", lambda e: e.dma_start(out=tri[:], in_=tri_d), writes=["tri"], dma="ldc")
        idb = P.sbuf("idb", [128, 128], BF16)
        P.op("sp", lambda e: e.dma_start(out=idb[:], in_=idb_d), writes=["idb"], dma="ldc")
        P.op("sp", lambda e: e.dma_start(out=sel[:], in_=sel_d), writes=["sel"], dma="ldc")
        P.op("dve", lambda e: e.memset(R[:], 0.0), writes=["R"])

        def load_head(h):
            s = h % 2
            P.op("sp", lambda e: e.dma_start(out=qT[s][:], in_=qT_d[:, h, :]), writes=[("q", s)], dma=f"lq{s}")
            P.op("sp", lambda e: e.dma_start(out=kT[s][:], in_=kT_d[:, h, :]), writes=[("k", s)], dma=f"lk{s}")
            P.op("sp", lambda e: e.dma_start(out=vv[s][:], in_=v_d[:, :, h * DH:(h + 1) * DH]),
                 writes=[("v", s)], dma=f"lv{s}")

        load_head(0)
        if kind == "fox":
            lf = P.sbuf("lf", [128, NCH, HL], F32)
            tri32 = P.sbuf("tri32", [128, 128], F32)
            ones32 = P.sbuf("ones32", [128, 128], F32)
            ctok = P.sbuf("ctok", [128, NCH, HL], F32)
            Ebc = P.sbuf("Ebc", [128, NQT, HL], F32)
            cT = P.sbuf("cT", [8, SEQ], F32)
            biasF = P.sbuf("biasF", [128, HL, NCH, NQT], F32)
            P.op("sp", lambda e: e.dma_start(out=lf[:], in_=logf_d), writes=["lf"], dma="ldc")
            P.op("sp", lambda e: e.dma_start(out=tri32[:], in_=tri32_d), writes=["tri32"], dma="ldc")
            P.op("sp", lambda e: e.dma_start(out=ones32[:], in_=ones32_d), writes=["ones32"], dma="ldc")
            for i in range(NCH):
                for i2 in range(i + 1):
                    P.op("pe", lambda e, i=i, i2=i2: e.matmul(
                        C.ps[:, 0, i * HL:(i + 1) * HL], lhsT=(ones32 if i2 < i else tri32)[:],
                        rhs=lf[:, i2, :], start=(i2 == 0), stop=(i2 == i)),
                        reads=["lf", "tri32", "ones32"], writes=[("ps", 0)])
            P.op("dve", lambda e: e.tensor_copy(out=ctok[:], in_=C.ps[:, 0, 0:NCH * HL].rearrange(
                "p (a b) -> p a b", b=HL)), reads=[("ps", 0)], writes=["ctok"])
            P.op("dve", lambda e: e.memset(Ebc[:], 0.0), writes=["Ebc"])
            for qt in range(1, NQT):
                for i2 in range(4 * qt):
                    P.op("pe", lambda e, qt=qt, i2=i2: e.matmul(
                        C.ps[:, 1, qt * HL:(qt + 1) * HL], lhsT=ones32[:], rhs=lf[:, i2, :],
                        start=(i2 == 0), stop=(i2 == 4 * qt - 1)),
                        reads=["lf", "ones32"], writes=[("ps", 1)])
            P.op("dve", lambda e: e.tensor_copy(out=Ebc[:, 1:NQT, :], in_=C.ps[:, 1, HL:NQT * HL].rearrange(
                "p (a b) -> p a b", b=HL)), reads=[("ps", 1)], writes=["Ebc"])
            for j in range(NCH):
                bk = 2 + j // 4
                for i2 in range(j + 1):
                    P.op("pe", lambda e, j=j, i2=i2, bk=bk: e.matmul(
                        C.ps[0:HL, bk, (j % 4) * 128:(j % 4 + 1) * 128], lhsT=lf[:, i2, :],
                        rhs=(ones32 if i2 < j else tri32)[:], start=(i2 == 0), stop=(i2 == j)),
                        reads=["lf", "tri32", "ones32"], writes=[("ps", bk)])
            for qt in range(NQT):
                P.op("dve", lambda e, qt=qt: e.tensor_copy(
                    out=cT[:, qt * TH:(qt + 1) * TH], in_=C.ps[0:HL, 2 + qt, :]),
                    reads=[("ps", 2 + qt)], writes=["cT"])
            P.op("dve", lambda e: e.tensor_copy(out=R[0:HL, 0:TH], in_=cT[:, 0:TH]),
                 reads=["cT"], writes=["R"])
            for qt in range(1, NQT):
                P.op("dve", lambda e, qt=qt: e.tensor_scalar(
                    out=R[0:HL, qt * TH:(qt + 1) * TH], in0=cT[:, qt * TH:(qt + 1) * TH],
                    scalar1=cT[:, qt * TH - 1:qt * TH], scalar2=None, op0=ALU.subtract),
                    reads=["cT"], writes=["R"])
            for h in range(HL):
                for i in range(NCH):
                    P.op("dve", lambda e, h=h, i=i: e.tensor_scalar(
                        out=biasF[:, h, i, :], in0=Ebc[:, :, h], scalar1=ctok[:, i, h:h + 1], scalar2=None,
                        op0=ALU.subtract), reads=["Ebc", "ctok"], writes=["biasF"])
        else:
            ident = P.sbuf("ident", [128, 128], F32)
            gmask = P.sbuf("gmask", [128, NCH, 8], F32)
            keep = P.sbuf("keep", [128, NCH, 8], F32)
            kq = P.sbuf("kq", [128, NCH, NQT], F32)
            shq = P.sbuf("shq", [8, SEQ], F32)
            slopes = P.sbuf("slopes", [128, HL], F32)
            biasM = P.sbuf("biasM", [128, NCH, NQT], F32)
            q32 = P.sbuf("q32", [128, SEQ], F32)
            km = P.sbuf("km", [128, 8], F32)
            gate = P.sbuf("gate", [128, NCH, 8], F32)
            top8 = P.sbuf("top8", [128, NCH, 8], F32)
            negm = P.sbuf("negm", [128, NCH, 8], F32)
            for nm, t_, d_ in (("ident", ident, ident_d), ("gmask", gmask, gmask_d), ("keep", keep, keep_d),
                               ("kq", kq, kq_d), ("shq", shq, shq_d), ("slopes", slopes, slope_d)):
                P.op("sp", lambda e, t_=t_, d_=d_: e.dma_start(out=t_[:], in_=d_), writes=[nm], dma="ldc")

        P.seal("ldc")
        for h in range(HL):
            s = h % 2
            if h + 1 < HL:
                load_head(h + 1)
            if kind == "moba":
                P.op("dve", lambda e, s=s: e.tensor_reduce(
                    out=km[:], in_=kT[s][:].rearrange("p (n k) -> p n k", k=256), axis=AX.X, op=ALU.add),
                    reads=[("k", s)], writes=["km"])
                P.op("act", lambda e, s=s: e.activation(out=q32[:], in_=qT[s][:], func=AF.Copy),
                     reads=[("q", s)], writes=["q32"])
                for tc in range(NCH):
                    P.op("pe", lambda e, tc=tc: e.matmul(
                        C.ps[:, 0, tc * 8:(tc + 1) * 8], lhsT=q32[:, tc * 128:(tc + 1) * 128], rhs=km[:],
                        start=True, stop=True), reads=["q32", "km"], writes=[("ps", 0)])
                P.op("dve", lambda e: e.tensor_tensor(
                    out=gate[:], in0=C.ps[:, 0, 0:NCH * 8].rearrange("p (a b) -> p a b", b=8), in1=gmask[:],
                    op=ALU.add), reads=[("ps", 0), "gmask"], writes=["gate"])
                for tc in range(NCH):
                    P.op("dve", lambda e, tc=tc: e.max(out=top8[:, tc, :], in_=gate[:, tc, :]),
                         reads=["gate"], writes=["top8"])
                for tc in range(NCH):
                    P.op("dve", lambda e, tc=tc: e.tensor_scalar(
                        out=negm[:, tc, :], in0=gate[:, tc, :], scalar1=top8[:, tc, 2:3], scalar2=NEG,
                        op0=ALU.is_lt, op1=ALU.mult), reads=["gate", "top8"], writes=["negm"])
                P.op("dve", lambda e: e.tensor_tensor(out=negm[:], in0=negm[:], in1=keep[:], op=ALU.mult),
                     reads=["negm", "keep"], writes=["negm"])
                for tc in range(NCH):
                    bk = 1 + (tc // 4) % 2
                    P.op("pe", lambda e, tc=tc, bk=bk: e.transpose(
                        out=C.ps[0:8, bk, (tc % 4) * 128:(tc % 4 + 1) * 128], in_=negm[:, tc, :],
                        identity=ident[:]), reads=["negm", "ident"], writes=[("ps", bk)])
                    if tc % 4 == 3:
                        qt = tc // 4
                        P.op("dve", lambda e, qt=qt, bk=bk, h=h: e.scalar_tensor_tensor(
                            out=R[0:8, qt * TH:(qt + 1) * TH], in0=shq[:, qt * TH:(qt + 1) * TH],
                            scalar=slopes[0:8, h:h + 1], in1=C.ps[0:8, bk, :], op0=ALU.mult, op1=ALU.add),
                            reads=[("ps", bk), "shq", "slopes"], writes=["R"])
                P.op("dve", lambda e, h=h: e.tensor_scalar(
                    out=biasM[:], in0=kq[:], scalar1=slopes[:, h:h + 1], scalar2=None, op0=ALU.mult),
                    reads=["kq", "slopes"], writes=["biasM"])
            for qt in range(NQT):
                ob, lb = 3 + (qt % 2), 5 + (qt % 2)
                nI = 4 * qt + 4
                for i in range(nI):
                    d = i - 4 * qt
                    c0 = d * 128 if d > 0 else 0
                    sb = 7 if (i % 2) else 0
                    pi = i % 3
                    q0 = qt * TH
                    if kind == "fox":
                        selT = sel[:, h * 128:(h + 1) * 128]
                        bias = biasF[:, h, i, qt:qt + 1]
                        bkeys = ["biasF"]
                    else:
                        selT = sel[:, (i // 2) * 128:(i // 2 + 1) * 128]
                        bias = biasM[:, i, qt:qt + 1]
                        bkeys = ["biasM"]
                    P.op("pe", lambda e, i=i, c0=c0, sb=sb, q0=q0, s=s: e.matmul(
                        C.ps[:, sb, c0:TH], lhsT=kT[s][:, i * 128:(i + 1) * 128], rhs=qT[s][:, q0 + c0:q0 + TH],
                        start=True, stop=False), reads=[("k", s), ("q", s)], writes=[("ps", sb)])
                    P.op("pe", lambda e, c0=c0, sb=sb, q0=q0, selT=selT, d=d: e.matmul(
                        C.ps[:, sb, c0:TH], lhsT=selT, rhs=R[:, q0 + c0:q0 + TH],
                        start=False, stop=(d < 0)), reads=["sel", "R"], writes=[("ps", sb)])
                    if d >= 0:
                        P.op("pe", lambda e, c0=c0, sb=sb: e.matmul(
                            C.ps[:, sb, c0:c0 + 128], lhsT=idb[:], rhs=tri[:], start=False, stop=True),
                            reads=["idb", "tri"], writes=[("ps", sb)])
                    P.op("act", lambda e, c0=c0, sb=sb, pi=pi, bias=bias: e.activation(
                        out=pT[pi][:, c0:TH], in_=C.ps[:, sb, c0:TH], func=AF.Exp, bias=bias),
                        reads=[("ps", sb)] + bkeys, writes=[("pT", pi)])
                    P.op("pe", lambda e, i=i, c0=c0, ob=ob, pi=pi, s=s, nI=nI: e.matmul(
                        C.ps[:, ob, c0:TH], lhsT=vv[s][:, i, :], rhs=pT[pi][:, c0:TH],
                        start=(i == 0), stop=(i == nI - 1)), reads=[("v", s), ("pT", pi)], writes=[("ps", ob)])
                    P.op("pe", lambda e, i=i, c0=c0, lb=lb, pi=pi, nI=nI: e.matmul(
                        C.ps[:, lb, c0:TH], lhsT=C.ones[:], rhs=pT[pi][:, c0:TH],
                        start=(i == 0), stop=(i == nI - 1)), reads=["ones", ("pT", pi)], writes=[("ps", lb)])
                P.op("dve", lambda e, lb=lb: e.reciprocal(out=rl[:], in_=C.ps[:, lb, :]),
                     reads=[("ps", lb)], writes=["rl"])
                P.op("dve", lambda e, ob=ob, qt=qt, s=s: e.tensor_tensor(
                    out=ao[s][:, qt * TH:(qt + 1) * TH], in0=C.ps[:, ob, :], in1=rl[:], op=ALU.mult),
                    reads=[("ps", ob), "rl"], writes=[("ao", s)])
            P.op("sp", lambda e, s=s, h=h: e.dma_start(out=out_d[:, h, :], in_=ao[s][:]),
                 reads=[("ao", s)], dma=f"sa{s}")
        P.barrier()
        P.emit()
    return nc


GL = 64
CL = 8
TWO_PI = 6.283185307179586
I32 = mybir.dt.int32


def build_s5():
    nc = bass.Bass("TRN2", target_bir_lowering=False)

    def din(name, shape, dt=F32):
        return nc.dram_tensor(name, list(shape), dt, kind="ExternalInput").ap()

    uT_d = din("uT", [128, CL, SEQ], BF16)
    A_d = {k: din(k, [128, CL * 64]) for k in ("areA", "aimA", "ldtA", "breA", "bimA")}
    B_d = {k: din(k, [128, GL]) for k in ("areB", "aimB", "ldtB")}
    cc1_d = din("cc1", [128, GL * 16])
    cc2_d = din("cc2", [128, GL * 16])
    dcol_d = din("dcol", [128, CL])
    tvec_d = din("tvec", [128, SEQ])
    bmask_d = din("bmask", [128, 8])
    ident_d = din("ident32", [128, 128])
    yT_d = nc.dram_tensor("yT", [128, CL, SEQ], BF16, kind="ExternalOutput").ap()

    P = Prog(nc)
    with P.stack:
        C = Ctx(P, wring=False)
        uT = P.sbuf("uT", [128, CL, SEQ], BF16)
        for j in range(CL):
            P.op("sp", lambda e, j=j: e.dma_start(out=uT[:, j, :], in_=uT_d[:, j, :]), writes=[("u", j)], dma="ldu")
        P.seal("ldu")
        cat1 = P.sbuf("cat1", [128, CL, 128], F32)
        cat2 = P.sbuf("cat2", [128, CL, 128], F32)
        cc1 = P.sbuf("cc1", [128, GL * 16], F32)
        cc2 = P.sbuf("cc2", [128, GL * 16], F32)
        rho = P.sbuf("rho", [128, GL], F32)
        omega = P.sbuf("omega", [128, GL], F32)
        dcol = P.sbuf("dcol", [128, CL], F32)
        tvec = P.sbuf("tvec", [128, SEQ], F32)
        bmask = P.sbuf("bmask", [128, 8], F32)
        ident = P.sbuf("ident", [128, 128], F32)
        diag = P.sbuf("diag", [128, CL, 128], BF16)
        halfpi = P.sbuf("halfpi", [128, 1], F32)
        P.op("dve", lambda e: e.memset(halfpi[:], TWO_PI / 4), writes=["halfpi"])
        for nm, t_, d_ in (("cc1", cc1, cc1_d), ("cc2", cc2, cc2_d), ("dcol", dcol, dcol_d), ("tvec", tvec, tvec_d),
                           ("bmask", bmask, bmask_d), ("ident", ident, ident_d)):
            P.op("sp", lambda e, t_=t_, d_=d_: e.dma_start(out=t_[:], in_=d_), writes=[nm], dma="ldc")

        with contextlib.ExitStack() as sub:
            def tmp(name, shape=(128, CL * 64), dt=F32):
                P.n_t += 1
                return sub.enter_context(nc.sbuf_tensor(f"{name}_{P.n_t}", list(shape), dt))

            A = {k: tmp(k) for k in A_d}
            for k in A_d:
                P.op("sp", lambda e, k=k: e.dma_start(out=A[k][:], in_=A_d[k]), writes=[k], dma="ldc")
            Bt = {k: tmp(k, (128, GL)) for k in B_d}
            for k in B_d:
                P.op("sp", lambda e, k=k: e.dma_start(out=Bt[k][:], in_=B_d[k]), writes=[k], dma="ldc")
            P.seal("ldc")
            cnt = [0]

            def dve2(out_, a, b, op, rd, wr):
                P.op("dve", lambda e: e.tensor_tensor(out=out_, in0=a, in1=b, op=op), reads=rd, writes=wr)

            def act1(out_, a, func, rd, wr, **kw):
                P.op("act", lambda e: e.activation(out=out_, in_=a, func=func, **kw), reads=rd, writes=wr)

            def dves(out_, a, s1, s2, op0, op1, rd, wr):
                if s2 is None:
                    P.op("dve", lambda e: e.tensor_scalar(out=out_, in0=a, scalar1=s1, scalar2=None, op0=op0),
                         reads=rd, writes=wr)
                else:
                    P.op("dve", lambda e: e.tensor_scalar(out=out_, in0=a, scalar1=s1, scalar2=s2, op0=op0, op1=op1),
                         reads=rd, writes=wr)

            def sincos(x, n, pre):
                ni = tmp(pre + "ni", (128, n), I32)
                nf = tmp(pre + "nf", (128, n))
                fr = tmp(pre + "fr", (128, n))
                sn = tmp(pre + "sn", (128, n))
                cs = tmp(pre + "cs", (128, n))
                k = pre
                P.op("dve", lambda e: e.tensor_copy(out=ni[:], in_=x), reads=[k + "x"], writes=[k + "ni"])
                P.op("dve", lambda e: e.tensor_copy(out=nf[:], in_=ni[:]), reads=[k + "ni"], writes=[k + "nf"])
                dve2(fr[:], x, nf[:], ALU.subtract, [k + "x", k + "nf"], [k + "fr"])
                act1(sn[:], fr[:], AF.Sin, [k + "fr"], [k + "sn"], scale=TWO_PI)
                act1(fr[:], fr[:], AF.Abs, [k + "fr", k + "sn"], [k + "fr"])
                act1(cs[:], fr[:], AF.Sin, [k + "fr", "halfpi"], [k + "cs"], scale=-TWO_PI, bias=halfpi[:])
                return sn, cs

            dtA = tmp("dtA"); xr = tmp("xr"); xt = tmp("xt"); er = tmp("er")
            act1(dtA[:], A["ldtA"][:], AF.Exp, ["ldtA"], ["dtA"])
            dve2(xr[:], A["areA"][:], dtA[:], ALU.mult, ["areA", "dtA"], ["xr"])
            dve2(xt[:], A["aimA"][:], dtA[:], ALU.mult, ["aimA", "dtA"], ["Ax"])
            dves(xt[:], xt[:], 1.0 / TWO_PI, None, ALU.mult, None, ["Ax"], ["Ax"])
            act1(er[:], xr[:], AF.Exp, ["xr"], ["er"])
            sn, cs = sincos(xt[:], CL * 64, "A")
            Ar = tmp("Ar"); Ai = tmp("Ai"); mag = tmp("mag"); t1 = tmp("t1"); t2 = tmp("t2")
            cr = tmp("cr"); ci = tmp("ci"); Br = tmp("Br"); Bi = tmp("Bi")
            dve2(Ar[:], er[:], cs[:], ALU.mult, ["er", "Acs"], ["Ar"])
            dve2(Ai[:], er[:], sn[:], ALU.mult, ["er", "Asn"], ["Ai"])
            dves(Ar[:], Ar[:], -1.0, None, ALU.add, None, ["Ar"], ["Ar"])
            dve2(mag[:], A["areA"][:], A["areA"][:], ALU.mult, ["areA"], ["mag"])
            dve2(t1[:], A["aimA"][:], A["aimA"][:], ALU.mult, ["aimA"], ["t1"])
            dve2(mag[:], mag[:], t1[:], ALU.add, ["mag", "t1"], ["mag"])
            P.op("dve", lambda e: e.reciprocal(out=mag[:], in_=mag[:]), reads=["mag"], writes=["mag"])
            dve2(t1[:], Ar[:], A["areA"][:], ALU.mult, ["Ar", "areA"], ["t1"])
            dve2(t2[:], Ai[:], A["aimA"][:], ALU.mult, ["Ai", "aimA"], ["t2"])
            dve2(cr[:], t1[:], t2[:], ALU.add, ["t1", "t2"], ["cr"])
            dve2(cr[:], cr[:], mag[:], ALU.mult, ["cr", "mag"], ["cr"])
            dve2(t1[:], Ai[:], A["areA"][:], ALU.mult, ["Ai", "areA", "cr"], ["t1"])
            dve2(t2[:], Ar[:], A["aimA"][:], ALU.mult, ["Ar", "aimA", "cr"], ["t2"])
            dve2(ci[:], t1[:], t2[:], ALU.subtract, ["t1", "t2"], ["ci"])
            dve2(ci[:], ci[:], mag[:], ALU.mult, ["ci", "mag"], ["ci"])
            dve2(t1[:], cr[:], A["breA"][:], ALU.mult, ["cr", "breA", "ci"], ["t1"])
            dve2(t2[:], ci[:], A["bimA"][:], ALU.mult, ["ci", "bimA"], ["t2"])
            dve2(Br[:], t1[:], t2[:], ALU.subtract, ["t1", "t2"], ["Br"])
            dve2(t1[:], cr[:], A["bimA"][:], ALU.mult, ["cr", "bimA", "Br"], ["t1"])
            dve2(t2[:], ci[:], A["breA"][:], ALU.mult, ["ci", "breA", "Br"], ["t2"])
            dve2(Bi[:], t1[:], t2[:], ALU.add, ["t1", "t2"], ["Bi"])
            Br3 = Br[:].rearrange("p (j q) -> p j q", q=64)
            Bi3 = Bi[:].rearrange("p (j q) -> p j q", q=64)
            P.op("dve", lambda e: e.tensor_copy(out=cat1[:, :, 0:64], in_=Br3), reads=["Br"], writes=["cat1"])
            P.op("dve", lambda e: e.tensor_copy(out=cat1[:, :, 64:128], in_=Bi3), reads=["Bi"], writes=["cat1"])
            P.op("dve", lambda e: e.tensor_copy(out=cat2[:, :, 0:64], in_=Bi3), reads=["Bi"], writes=["cat2"])
            P.op("dve", lambda e: e.tensor_scalar(out=cat2[:, :, 64:128], in0=Br3, scalar1=-1.0, scalar2=None,
                                                  op0=ALU.mult), reads=["Br"], writes=["cat2"])
            dtB = tmp("dtB", (128, GL)); xb = tmp("xb", (128, GL))
            act1(dtB[:], Bt["ldtB"][:], AF.Exp, ["ldtB"], ["dtB"])
            dve2(xb[:], Bt["areB"][:], dtB[:], ALU.mult, ["areB", "dtB"], ["xb"])
            act1(rho[:], xb[:], AF.Exp, ["xb"], ["rho"])
            dve2(omega[:], Bt["aimB"][:], dtB[:], ALU.mult, ["aimB", "dtB"], ["omega"])
            dves(omega[:], omega[:], 1.0 / TWO_PI, None, ALU.mult, None, ["omega"], ["omega"])
            dves(cc1[64:128, :], cc1[64:128, :], -1.0, None, ALU.mult, None, ["cc1"], ["cc1"])
            dves(cc2[:], cc2[:], -1.0, None, ALU.mult, None, ["cc2"], ["cc2"])
            for j in range(CL):
                P.op("dve", lambda e, j=j: e.tensor_scalar(out=diag[:, j, :], in0=ident[:], scalar1=dcol[:, j:j + 1],
                                                           scalar2=None, op0=ALU.mult),
                     reads=["ident", "dcol"], writes=["diag"])
            P.barrier()

        W1 = [P.sbuf("W1", [128, 8, 128], BF16) for _ in range(2)]
        W2 = [P.sbuf("W2", [128, 8, 128], BF16) for _ in range(2)]
        Cm1 = [P.sbuf("Cm1", [128, 8, 128], BF16) for _ in range(2)]
        Cm2 = [P.sbuf("Cm2", [128, 8, 128], BF16) for _ in range(2)]
        xph = P.sbuf("xph", [128, SEQ], F32)
        ni = P.sbuf("ni", [128, SEQ], I32)
        nf = P.sbuf("nf", [128, SEQ], F32)
        SIN = [P.sbuf("SIN", [128, SEQ], F32) for _ in range(2)]
        COS = [P.sbuf("COS", [128, SEQ], F32) for _ in range(2)]
        bt = P.sbuf("bt", [128, SEQ], F32)
        G = P.sbuf("G", [128, SEQ], F32)
        V1 = P.sbuf("V1", [128, SEQ], BF16)
        V2 = P.sbuf("V2", [128, SEQ], BF16)
        ta = [P.sbuf("ta", [128, TH], F32) for _ in range(2)]
        tb = [P.sbuf("tb", [128, TH], F32) for _ in range(2)]
        yst = [P.sbuf("yst", [128, SEQ], BF16) for _ in range(2)]
        g1 = [P.sbuf("g1", [128, TH], F32) for _ in range(2)]
        g2 = [P.sbuf("g2", [128, TH], F32) for _ in range(2)]
        for s_ in range(2):
            P.op("dve", lambda e, s_=s_: e.memset(Cm1[s_][:], 0.0), writes=[("Cm1", s_)])
            P.op("dve", lambda e, s_=s_: e.memset(Cm2[s_][:], 0.0), writes=[("Cm2", s_)])
        cc1v = cc1[:].rearrange("p (j i c) -> p j i c", i=8, c=16)
        cc2v = cc2[:].rearrange("p (j i c) -> p j i c", i=8, c=16)
        for j in range(CL):
            s = j % 2
            for i in range(8):
                P.op("dve", lambda e, j=j, i=i, s=s: e.tensor_scalar(
                    out=W1[s][:, i, :], in0=cat1[:, j, :], scalar1=bmask[:, i:i + 1], scalar2=None, op0=ALU.mult),
                    reads=["cat1", "bmask"], writes=[("W1", s)])
                P.op("dve", lambda e, j=j, i=i, s=s: e.tensor_scalar(
                    out=W2[s][:, i, :], in0=cat2[:, j, :], scalar1=bmask[:, i:i + 1], scalar2=None, op0=ALU.mult),
                    reads=["cat2", "bmask"], writes=[("W2", s)])
                P.op("dve", lambda e, j=j, i=i, s=s: e.tensor_copy(
                    out=Cm1[s][:, i, 16 * i:16 * i + 16], in_=cc1v[:, j, i, :]),
                    reads=["cc1"], writes=[("Cm1", s)])
                P.op("dve", lambda e, j=j, i=i, s=s: e.tensor_copy(
                    out=Cm2[s][:, i, 16 * i:16 * i + 16], in_=cc2v[:, j, i, :]),
                    reads=["cc2"], writes=[("Cm2", s)])
            for tt in range(NQT):
                P.op("pe", lambda e, j=j, tt=tt: e.matmul(
                    C.ps[:, tt, :], lhsT=diag[:, j, :], rhs=uT[:, j, tt * TH:(tt + 1) * TH], start=True, stop=False),
                    reads=["diag", ("u", j)], writes=[("ps", tt)])
            for i in range(8):
                g = j * 8 + i
                tsel = g % 2
                P.op("pool", lambda e, g=g: e.tensor_scalar(
                    out=xph[:], in0=tvec[:], scalar1=omega[:, g:g + 1], scalar2=0.0, op0=ALU.mult, op1=ALU.add),
                    reads=["tvec", "omega"], writes=["xph"])
                P.op("pool", lambda e: e.tensor_copy(out=ni[:], in_=xph[:]), reads=["xph"], writes=["ni"])
                P.op("pool", lambda e: e.tensor_copy(out=nf[:], in_=ni[:]), reads=["ni"], writes=["nf"])
                P.op("pool", lambda e: e.tensor_tensor(out=xph[:], in0=xph[:], in1=nf[:], op=ALU.subtract),
                     reads=["xph", "nf"], writes=["xph"])
                P.op("act", lambda e, tsel=tsel: e.activation(out=SIN[tsel][:], in_=xph[:], func=AF.Sin, scale=TWO_PI),
                     reads=["xph"], writes=[("SIN", tsel)])
                P.op("act", lambda e: e.activation(out=nf[:], in_=xph[:], func=AF.Abs), reads=["xph"], writes=["nf"])
                P.op("act", lambda e, tsel=tsel: e.activation(out=COS[tsel][:], in_=nf[:], func=AF.Sin, scale=-TWO_PI,
                                                             bias=halfpi[:]),
                     reads=["nf", "halfpi"], writes=[("COS", tsel)])
                for tt in range(NQT):
                    b1, b2 = 4 + (tt % 2), 6 + (tt % 2)
                    sl = slice(tt * TH, (tt + 1) * TH)
                    P.op("pe", lambda e, i=i, s=s, j=j, sl=sl, b1=b1: e.matmul(
                        C.ps[:, b1, :], lhsT=W1[s][:, i, :], rhs=uT[:, j, sl], start=True, stop=True),
                        reads=[("W1", s), ("u", j)], writes=[("ps", b1)])
                    P.op("pe", lambda e, i=i, s=s, j=j, sl=sl, b2=b2: e.matmul(
                        C.ps[:, b2, :], lhsT=W2[s][:, i, :], rhs=uT[:, j, sl], start=True, stop=True),
                        reads=[("W2", s), ("u", j)], writes=[("ps", b2)])
                    x = tt % 2
                    P.op("dve", lambda e, sl=sl, b1=b1, x=x, tsel=tsel: e.tensor_tensor(
                        out=ta[x][:], in0=C.ps[:, b1, :], in1=COS[tsel][:, sl], op=ALU.mult),
                        reads=[("ps", b1), ("COS", tsel)], writes=[("ta", x)])
                    P.op("dve", lambda e, sl=sl, b2=b2, x=x, tsel=tsel: e.tensor_tensor(
                        out=tb[x][:], in0=C.ps[:, b2, :], in1=SIN[tsel][:, sl], op=ALU.mult),
                        reads=[("ps", b2), ("SIN", tsel)], writes=[("tb", x)])
                    P.op("dve", lambda e, sl=sl, x=x: e.tensor_tensor(
                        out=bt[:, sl], in0=ta[x][:], in1=tb[x][:], op=ALU.add),
                        reads=[("ta", x), ("tb", x)], writes=["bt"])
                P.op("dve", lambda e, g=g: e.tensor_tensor_scan(
                    out=G[:], data0=rho[:, g:g + 1].broadcast_to([128, SEQ]), data1=bt[:], initial=0.0,
                    op0=ALU.mult, op1=ALU.add), reads=["rho", "bt"], writes=["G"])
                P.op("dve", lambda e, tsel=tsel: e.tensor_tensor(out=V1[:], in0=G[:], in1=COS[tsel][:], op=ALU.mult),
                     reads=["G", ("COS", tsel)], writes=["V1"])
                P.op("dve", lambda e, tsel=tsel: e.tensor_tensor(out=V2[:], in0=G[:], in1=SIN[tsel][:], op=ALU.mult),
                     reads=["G", ("SIN", tsel)], writes=["V2"])
                for tt in range(NQT):
                    sl = slice(tt * TH, (tt + 1) * TH)
                    P.op("pe", lambda e, i=i, s=s, tt=tt, sl=sl: e.matmul(
                        C.ps[:, tt, :], lhsT=Cm1[s][:, i, :], rhs=V1[:, sl], start=False, stop=False),
                        reads=[("Cm1", s), "V1"], writes=[("ps", tt)])
                    P.op("pe", lambda e, i=i, s=s, tt=tt, sl=sl: e.matmul(
                        C.ps[:, tt, :], lhsT=Cm2[s][:, i, :], rhs=V2[:, sl], start=False, stop=(i == 7)),
                        reads=[("Cm2", s), "V2"], writes=[("ps", tt)])
            ys = yst[j % 2]
            for tt in range(NQT):
                x = tt % 2
                sl = slice(tt * TH, (tt + 1) * TH)
                P.op("act", lambda e, tt=tt, x=x: e.activation(out=g1[x][:], in_=C.ps[:, tt, :], func=AF.Square),
                     reads=[("ps", tt)], writes=[("g1", x)])
                P.op("dve", lambda e, x=x: e.tensor_scalar(out=g1[x][:], in0=g1[x][:], scalar1=0.044715, scalar2=1.0,
                                                           op0=ALU.mult, op1=ALU.add),
                     reads=[("g1", x)], writes=[("g1", x)])
                P.op("dve", lambda e, tt=tt, x=x: e.tensor_tensor(out=g1[x][:], in0=C.ps[:, tt, :], in1=g1[x][:],
                                                                   op=ALU.mult),
                     reads=[("ps", tt), ("g1", x)], writes=[("g1", x)])
                P.op("act", lambda e, x=x: e.activation(out=g2[x][:], in_=g1[x][:], func=AF.Sigmoid,
                                                        scale=1.5957691216057308),
                     reads=[("g1", x)], writes=[("g2", x)])
                P.op("dve", lambda e, tt=tt, x=x, sl=sl, ys=ys: e.tensor_tensor(
                    out=ys[:, sl], in0=C.ps[:, tt, :], in1=g2[x][:], op=ALU.mult),
                    reads=[("ps", tt), ("g2", x)], writes=[("yst", j % 2)])
            P.op("sp", lambda e, j=j, ys=ys: e.dma_start(out=yT_d[:, j, :], in_=ys[:]),
                 reads=[("yst", j % 2)], dma=f"sy{j % 2}")
        P.barrier()
        P.emit()
    return nc


import ml_dtypes

NCORES = 8
_PROGS = {}
_DBG = None


def _prog(key, builder):
    if key not in _PROGS:
        _PROGS[key] = builder()
    return _PROGS[key]


def _c(a):
    return np.ascontiguousarray(a)


def tile_w(W, gc):
    K, N = W.shape
    kc = K // 128
    return _c(W.reshape(kc, 128, N // gc, gc).transpose(2, 1, 0, 3)).reshape(N // gc, 128, kc * gc)


def lay_ffn_in(w):
    wg = w[:, :FF].reshape(D, FC, 128)
    wu = w[:, FF:].reshape(D, FC, 128)
    return tile_w(np.concatenate([wg, wu], axis=2).reshape(D, FC * 256), 256)


def lay_ffn_out(w):
    return _c(w.reshape(NQ, FQ, 128, 8, 256).transpose(0, 3, 2, 1, 4)).reshape(NQ, 8, 128, FQ * 256)


def lay_glu(w):
    wv = w[:, :D].reshape(D, KC, 128)
    wg = w[:, D:].reshape(D, KC, 128)
    return tile_w(np.concatenate([wv, wg], axis=2).reshape(D, KC * 256), 256)


def fm(x):
    t, f = x.shape
    return _c(x.T.reshape(f // 128, 128, t).transpose(1, 0, 2))


def unfm(a):
    p, c, t = a.shape
    return _c(a.transpose(2, 1, 0)).reshape(t, c * 128)


def lay_gains(gs):
    return _c(np.concatenate([g.reshape(KC, 128).T for g in gs], axis=1))


def _run(nc, in_maps):
    res = run_bass_kernel_spmd(nc, in_maps, core_ids=list(range(NCORES)))
    return res.results


def _attn_consts(kind, hh):
    k = np.arange(128)
    c = {}
    c["trineg"] = np.where(k[:, None] <= k[None, :], 0.0, NEG).astype(ml_dtypes.bfloat16)
    c["ident_bf"] = np.eye(128, dtype=np.float32).astype(ml_dtypes.bfloat16)
    sel = np.zeros((128, 8, 128), np.float32)
    for n in range(8):
        sel[n, n, :] = 1.0
    c["sel"] = sel.reshape(128, 8 * 128).astype(ml_dtypes.bfloat16)
    if kind == "fox":
        c["tri32"] = (k[:, None] <= k[None, :]).astype(np.float32)
        c["ones32"] = np.ones((128, 128), np.float32)
    else:
        c["ident32"] = np.eye(128, dtype=np.float32)
        own = (np.arange(NCH) // 2)[:, None]
        n = np.arange(8)[None, :]
        valid = (n < own)
        c["gmask"] = _c(np.broadcast_to(np.where(valid, 0.0, -1e30).astype(np.float32), (128, NCH, 8)))
        c["keep"] = _c(np.broadcast_to(valid.astype(np.float32), (128, NCH, 8)))
        kpos = (np.arange(NCH)[None, :, None] * 128 + k[:, None, None]).astype(np.float32)
        c["kq"] = _c(kpos - 512.0 * np.arange(NQT)[None, None, :]).astype(np.float32)
        c["shq"] = _c(np.broadcast_to(-(np.arange(SEQ) % TH).astype(np.float32), (8, SEQ)))
        hs = hh * HL + np.arange(HL)
        slopes = (2.0 ** (-8.0 * (hs + 1) / H)).astype(np.float32)
        c["slopes"] = _c(np.broadcast_to(slopes, (128, HL)))
    return c


def kernel(x, p, norm_g, final_g, w_ffn_in, w_ffn_out, w_ple_gate, w_ple_proj,
           fox_w_in, fox_b_f, fox_w_o, moba_w_qkv, moba_w_o,
           s5_w_in, s5_a_re, s5_a_im, s5_log_dt, s5_b_re, s5_b_im,
           s5_c_re, s5_c_im, s5_d, s5_w_glu):
    f32 = np.float32
    bf = ml_dtypes.bfloat16
    x = np.asarray(x, f32)
    p = np.asarray(p, f32)
    norm_g = np.asarray(norm_g, f32)
    cores = [(c // 2, c % 2) for c in range(NCORES)]
    tok = lambda s: slice(s * T, (s + 1) * T)
    kinds = ["fox", "moba", "s5", "fox"]
    h_fm = [fm(x[b, tok(s)]) for b, s in cores]
    mix = None
    out = None
    for step in range(DEPTH + 1):
        kind_in = None if step == 0 else ("s5" if kinds[step - 1] == "s5" else "attn")
        kind_out = "final" if step == DEPTH else kinds[step]
        gl = []
        shared = {}
        if kind_in is not None:
            L = step - 1
            j = L // 3
            gl += [norm_g[L, 2], norm_g[L, 3]]
            if kind_in == "attn":
                wo = np.asarray(fox_w_o[j] if kinds[L] == "fox" else moba_w_o[j], f32)
                shared["wo"] = tile_w(wo, 256)
            else:
                shared["wglu"] = lay_glu(np.asarray(s5_w_glu[j], f32))
            shared["f2_in"] = lay_ffn_in(np.asarray(w_ffn_in[L, 1], f32))
            shared["f2_out"] = lay_ffn_out(np.asarray(w_ffn_out[L, 1], f32))
            shared["ple_g"] = tile_w(np.asarray(w_ple_gate[L], f32), 256)
            shared["ple_p"] = tile_w(np.asarray(w_ple_proj[L], f32), D)
        if kind_out == "final":
            gl += [np.asarray(final_g, f32)]
        else:
            L2 = step
            j2 = L2 // 3
            gl += [norm_g[L2, 0], norm_g[L2, 1]]
            shared["f1_in"] = lay_ffn_in(np.asarray(w_ffn_in[L2, 0], f32))
            shared["f1_out"] = lay_ffn_out(np.asarray(w_ffn_out[L2, 0], f32))
            if kind_out in ("fox", "moba"):
                w = np.asarray(fox_w_in[j2] if kind_out == "fox" else moba_w_qkv[j2], f32)
                shared["wq"] = tile_w(w[:, 0:D], 256)
                shared["wk"] = tile_w(w[:, D:2 * D], 256)
                shared["wv"] = tile_w(w[:, 2 * D:3 * D], 256)
                if kind_out == "fox":
                    shared["wf"] = tile_w(_c(w[:, 3 * D:3 * D + H]), H)
                    shared["bf_bc"] = _c(np.broadcast_to(np.asarray(fox_b_f[j2], f32), (128, H)))
            else:
                shared["wu"] = tile_w(np.asarray(s5_w_in[j2], f32), 256)
        shared["gains"] = lay_gains(gl)
        nc = _prog(("R", kind_in, kind_out), lambda: build_R(kind_in, kind_out, len(gl)))
        in_maps = []
        for ci, (b, s) in enumerate(cores):
            m = dict(shared)
            m["h_in"] = h_fm[ci]
            if kind_in is not None:
                m["mix_in"] = mix[ci]
                m["pT"] = fm(p[step - 1, b, tok(s)])
            in_maps.append(m)
        res = _run(nc, in_maps)
        del shared, in_maps
        if kind_out == "final":
            out = np.zeros((BATCH, SEQ, D), f32)
            for ci, (b, s) in enumerate(cores):
                out[b, tok(s)] = unfm(np.asarray(res[ci]["out"], f32))
            break
        h_fm = [np.asarray(res[ci]["h_out"], f32) for ci in range(NCORES)]
        if _DBG is not None:
            _DBG[f"h_{step}"] = [a.copy() for a in h_fm[:2]]
            _DBG[f"res_{step}"] = [dict(res[0]), dict(res[1])]
        in_maps = []
        if kind_out in ("fox", "moba"):
            for ci, (b, hh) in enumerate(cores):
                r0, r1 = res[2 * b], res[2 * b + 1]
                m = _attn_consts(kind_out, hh)
                hs = slice(hh * HL, (hh + 1) * HL)
                m["qT"] = _c(np.concatenate([r0["qT"][:, hs, :], r1["qT"][:, hs, :]], axis=2))
                m["kT"] = _c(np.concatenate([r0["kT"][:, hs, :], r1["kT"][:, hs, :]], axis=2))
                fs = slice(hh * HL * DH, (hh + 1) * HL * DH)
                m["v"] = _c(np.concatenate([r0["v"][:, :, fs], r1["v"][:, :, fs]], axis=1))
                if kind_out == "fox":
                    m["logf"] = _c(np.concatenate([r0["logf"][:, :, hs], r1["logf"][:, :, hs]], axis=1))
                in_maps.append(m)
            ncm = _prog(("M", kind_out), lambda: build_attn(kind_out))
            mres = _run(ncm, in_maps)
            mix = []
            for ci, (b, s) in enumerate(cores):
                a0, a1 = mres[2 * b]["attnT"], mres[2 * b + 1]["attnT"]
                mix.append(_c(np.concatenate([a0[:, :, tok(s)], a1[:, :, tok(s)]], axis=1)))
        else:
            j2 = step // 3
            a_re = np.asarray(s5_a_re[j2], f32); a_im = np.asarray(s5_a_im[j2], f32)
            ldt = np.asarray(s5_log_dt[j2], f32)
            b_re = np.asarray(s5_b_re[j2], f32); b_im = np.asarray(s5_b_im[j2], f32)
            c_re = np.asarray(s5_c_re[j2], f32); c_im = np.asarray(s5_c_im[j2], f32)
            dsk = np.asarray(s5_d[j2], f32)
            k = np.arange(128)
            for ci, (b, hh) in enumerate(cores):
                r0, r1 = res[2 * b], res[2 * b + 1]
                gs = slice(hh * GL, (hh + 1) * GL)
                m = {}
                cs = slice(hh * CL, (hh + 1) * CL)
                m["uT"] = _c(np.concatenate([r0["uT"][:, cs, :], r1["uT"][:, cs, :]], axis=2))

                def layA(a):
                    a4 = a.reshape(8, 8, 1, 64).transpose(1, 2, 0, 3)
                    return _c(np.broadcast_to(a4, (8, 16, 8, 64))).reshape(128, CL * 64)

                m["areA"] = layA(a_re[gs])
                m["aimA"] = layA(a_im[gs])
                m["ldtA"] = layA(np.broadcast_to(ldt[gs][:, None], (GL, 64)))

                def layBm(bm):
                    return _c(bm.reshape(8, 8, 64, 16).transpose(1, 3, 0, 2)).reshape(128, CL * 64)

                m["breA"] = layBm(b_re[gs])
                m["bimA"] = layBm(b_im[gs])

                def layB(a):
                    return _c(np.concatenate([a.T, a.T], axis=0))

                m["areB"] = layB(a_re[gs])
                m["aimB"] = layB(a_im[gs])
                m["ldtB"] = _c(np.broadcast_to(ldt[gs][None, :], (128, GL)))
                cr = c_re[gs].transpose(2, 0, 1)
                cim = c_im[gs].transpose(2, 0, 1)
                m["cc1"] = _c(np.concatenate([cr, cim], axis=0)).reshape(128, GL * 16)
                m["cc2"] = _c(np.concatenate([cim, cr], axis=0)).reshape(128, GL * 16)
                m["dcol"] = _c(dsk[hh * CL * 128:(hh + 1) * CL * 128].reshape(CL, 128).T)
                m["tvec"] = _c(np.broadcast_to(np.arange(SEQ, dtype=f32), (128, SEQ)))
                m["bmask"] = (k[:, None] // 16 == np.arange(8)[None, :]).astype(f32)
                m["ident32"] = np.eye(128, dtype=f32)
                in_maps.append(m)
            ncm = _prog(("M", "s5"), build_s5)
            mres = _run(ncm, in_maps)
            mix = []
            for ci, (b, s) in enumerate(cores):
                y0, y1 = mres[2 * b]["yT"], mres[2 * b + 1]["yT"]
                mix.append(_c(np.concatenate([y0[:, :, tok(s)], y1[:, :, tok(s)]], axis=1)))
        if _DBG is not None:
            _DBG[f"mix_{step}"] = [np.asarray(a, f32) for a in mix[:2]]
        del in_maps
    return out
```
